# Optimizing a Trainium2 kernel written in Bass

```python
import jax, jax.numpy as jnp
from jax import lax
import numpy as np

D_MODEL = 2048
BATCH = 1
SEQ = 8192
DEPTH = 1

CHUNK = 64
MEM_LEN = 256
MIX_WIDTH = D_MODEL
POOL_WIDTH = MIX_WIDTH // 2
CONV_WIDTH = MIX_WIDTH - POOL_WIDTH
POOL_WINDOWS = (2, 4, 8, 16)
N_POOL_GROUPS = len(POOL_WINDOWS)
POOL_GROUP_DIM = POOL_WIDTH // N_POOL_GROUPS
CONV_KERNEL = 31
XATTN_HEADS = 4
XATTN_HEAD_DIM = D_MODEL // XATTN_HEADS
N_GROUPS = 4
EXPERTS_PER_GROUP = 8
N_EXPERTS = N_GROUPS * EXPERTS_PER_GROUP
TOP_K = 2
D_EXPERT = D_MODEL // 4
MOE_BLOCK = 128
EPS = 1e-6

kernel_name = "hymba_pool_conformer_xmem_hmoe"


def rmsnorm(x, g):
    xf = x.astype(jnp.float32)
    y = xf * lax.rsqrt(jnp.mean(xf * xf, axis=-1, keepdims=True) + EPS)
    return (y * g.astype(jnp.float32)).astype(x.dtype)


def layernorm(x, g, b):
    xf = x.astype(jnp.float32)
    mu = jnp.mean(xf, axis=-1, keepdims=True)
    var = jnp.mean(jnp.square(xf - mu), axis=-1, keepdims=True)
    y = (xf - mu) * lax.rsqrt(var + EPS)
    return (y * g.astype(jnp.float32) + b.astype(jnp.float32)).astype(x.dtype)


def pool_mixer(u, w_pool, pool_scale):
    B, S, _ = u.shape
    ug = u.reshape(B, S, N_POOL_GROUPS, POOL_GROUP_DIM)
    cs = jnp.cumsum(ug.astype(jnp.float32), axis=1)
    cs0 = jnp.concatenate([jnp.zeros_like(cs[:, :1]), cs], axis=1)
    pos = jnp.arange(S)
    outs = []
    for g, w in enumerate(POOL_WINDOWS):
        c = cs0[:, :, g]
        lo = jnp.pad(c, ((0, 0), (w - 1, 0), (0, 0)))[:, :S]
        wsum = c[:, 1:] - lo
        cnt = jnp.minimum(pos + 1, w).astype(jnp.float32)[None, :, None]
        outs.append(wsum / cnt - ug[:, :, g].astype(jnp.float32))
    d = jnp.stack(outs, axis=2).astype(u.dtype)
    z = jnp.einsum('bsgc,gcd->bsgd', d, w_pool)
    return z.reshape(B, S, POOL_WIDTH) * pool_scale


def conv_module(v, w_dw, b_dw, ln_g, ln_b, w_pw):
    a, gate = jnp.split(v, 2, axis=-1)
    y = a * jax.nn.sigmoid(gate)
    y = lax.conv_general_dilated(y, w_dw, window_strides=(1,), padding=[(CONV_KERNEL - 1, 0)],
                                 dimension_numbers=('NWC', 'WIO', 'NWC'),
                                 feature_group_count=CONV_WIDTH)
    y = layernorm(y + b_dw, ln_g, ln_b)
    y = jax.nn.silu(y)
    return y @ w_pw


def cross_attention(h, m, w_q, w_kv, w_o):
    B, S, D = h.shape
    M = m.shape[1]
    q = (h @ w_q).reshape(B, S, XATTN_HEADS, XATTN_HEAD_DIM)
    k, v = jnp.split(m @ w_kv, 2, axis=-1)
    k = k.reshape(B, M, XATTN_HEADS, XATTN_HEAD_DIM)
    v = v.reshape(B, M, XATTN_HEADS, XATTN_HEAD_DIM)
    s = jnp.einsum('bshd,bmhd->bhsm', q, k).astype(jnp.float32) * (XATTN_HEAD_DIM ** -0.5)
    p = jax.nn.softmax(s, axis=-1).astype(v.dtype)
    o = jnp.einsum('bhsm,bmhd->bshd', p, v).reshape(B, S, D)
    return o @ w_o


def hierarchical_moe(h, w_rg, b_rg, w_re, b_re, w_gate, w_up, w_down):
    B, S, D = h.shape
    N = B * S
    hf = h.reshape(N, D)
    lg = (hf @ w_rg).astype(jnp.float32) + b_rg.astype(jnp.float32)
    pg = jax.nn.softmax(lg, axis=-1)
    pg_top, g_idx = lax.top_k(pg, 1)
    le = ((hf @ w_re).astype(jnp.float32) + b_re.astype(jnp.float32)).reshape(N, N_GROUPS, EXPERTS_PER_GROUP)
    le_sel = jnp.take_along_axis(le, g_idx[:, :, None], axis=1)[:, 0]
    pe = jax.nn.softmax(le_sel, axis=-1)
    pe_top, e_loc = lax.top_k(pe, TOP_K)
    w_tok = pg_top * pe_top / jnp.sum(pe_top, axis=-1, keepdims=True)
    e_glob = g_idx * EXPERTS_PER_GROUP + e_loc
    A = N * TOP_K
    flat_e = e_glob.reshape(A)
    flat_tok = jnp.repeat(jnp.arange(N, dtype=jnp.int32), TOP_K)
    flat_w = w_tok.reshape(A)
    order = jnp.argsort(flat_e)
    se, st, sw = flat_e[order], flat_tok[order], flat_w[order]
    counts = jnp.bincount(flat_e, length=N_EXPERTS)
    starts = jnp.cumsum(counts) - counts
    padded = (counts + MOE_BLOCK - 1) // MOE_BLOCK * MOE_BLOCK
    pends = jnp.cumsum(padded)
    pstarts = pends - padded
    dest = pstarts[se] + (jnp.arange(A) - starts[se])
    nb = (A + MOE_BLOCK - 1) // MOE_BLOCK + N_EXPERTS
    P = nb * MOE_BLOCK
    slot_tok = jnp.zeros((P,), jnp.int32).at[dest].set(st)
    slot_w = jnp.zeros((P,), jnp.float32).at[dest].set(sw)
    block_e = jnp.clip(jnp.searchsorted(pends, jnp.arange(nb) * MOE_BLOCK, side='right'), 0, N_EXPERTS - 1)
    xs = hf[slot_tok].reshape(nb, MOE_BLOCK, D)

    def expert_block(args):
        xb, e = args
        hid = jax.nn.silu(xb @ w_gate[e]) * (xb @ w_up[e])
        return hid @ w_down[e]

    ys = lax.map(expert_block, (xs, block_e)).reshape(P, D)
    ys = ys * slot_w[:, None].astype(ys.dtype)
    out = jnp.zeros((N, D), h.dtype).at[slot_tok].add(ys)
    return out.reshape(B, S, D)


def setup_inputs(seed: int = 0) -> dict:
    key = jax.random.key(seed)
    ks = jax.random.split(key, 32)
    f32 = jnp.float32
    L = DEPTH

    def w(k, shape, fan_in):
        return jax.random.normal(k, shape, f32) * (fan_in ** -0.5)

    def gain(k, shape):
        return 1.0 + 0.02 * jax.random.normal(k, shape, f32)

    return {
        "x": jax.random.normal(ks[0], (BATCH, SEQ, D_MODEL), f32),
        "mem": jax.random.normal(ks[1], (BATCH, MEM_LEN, D_MODEL), f32),
        "norm_mix_g": gain(ks[2], (L, D_MODEL)),
        "w_in": w(ks[3], (L, D_MODEL, POOL_WIDTH + 2 * CONV_WIDTH), D_MODEL),
        "w_pool": w(ks[4], (L, N_POOL_GROUPS, POOL_GROUP_DIM, POOL_GROUP_DIM), POOL_GROUP_DIM),
        "pool_scale": gain(ks[5], (L, POOL_WIDTH)),
        "w_dw": w(ks[6], (L, CONV_KERNEL, 1, CONV_WIDTH), CONV_KERNEL),
        "b_dw": 0.02 * jax.random.normal(ks[7], (L, CONV_WIDTH), f32),
        "conv_ln_g": gain(ks[8], (L, CONV_WIDTH)),
        "conv_ln_b": 0.02 * jax.random.normal(ks[9], (L, CONV_WIDTH), f32),
        "w_conv_pw": w(ks[10], (L, CONV_WIDTH, CONV_WIDTH), CONV_WIDTH),
        "out_norm_a_g": gain(ks[11], (L, POOL_WIDTH)),
        "out_norm_b_g": gain(ks[12], (L, CONV_WIDTH)),
        "w_out": w(ks[13], (L, MIX_WIDTH, D_MODEL), MIX_WIDTH),
        "norm_xattn_g": gain(ks[14], (L, D_MODEL)),
        "norm_mem_g": gain(ks[15], (L, D_MODEL)),
        "w_q_mem": w(ks[16], (L, D_MODEL, D_MODEL), D_MODEL),
        "w_kv_mem": w(ks[17], (L, D_MODEL, 2 * D_MODEL), D_MODEL),
        "w_o_mem": w(ks[18], (L, D_MODEL, D_MODEL), D_MODEL),
        "norm_ffn_g": gain(ks[19], (L, D_MODEL)),
        "w_router_group": w(ks[20], (L, D_MODEL, N_GROUPS), D_MODEL),
        "b_router_group": 0.01 * jax.random.normal(ks[21], (L, N_GROUPS), f32),
        "w_router_expert": w(ks[22], (L, D_MODEL, N_EXPERTS), D_MODEL),
        "b_router_expert": 0.01 * jax.random.normal(ks[23], (L, N_EXPERTS), f32),
        "w_exp_gate": w(ks[24], (L, N_EXPERTS, D_MODEL, D_EXPERT), D_MODEL),
        "w_exp_up": w(ks[25], (L, N_EXPERTS, D_MODEL, D_EXPERT), D_MODEL),
        "w_exp_down": w(ks[26], (L, N_EXPERTS, D_EXPERT, D_MODEL), D_EXPERT),
        "final_norm_g": gain(ks[27], (D_MODEL,)),
    }


def reference(x, mem, norm_mix_g, w_in, w_pool, pool_scale, w_dw, b_dw, conv_ln_g, conv_ln_b,
              w_conv_pw, out_norm_a_g, out_norm_b_g, w_out, norm_xattn_g, norm_mem_g, w_q_mem,
              w_kv_mem, w_o_mem, norm_ffn_g, w_router_group, b_router_group, w_router_expert,
              b_router_expert, w_exp_gate, w_exp_up, w_exp_down, final_norm_g):
    for l in range(DEPTH):
        h = rmsnorm(x, norm_mix_g[l])
        proj = h @ w_in[l]
        ya = pool_mixer(proj[..., :POOL_WIDTH], w_pool[l], pool_scale[l])
        yb = conv_module(proj[..., POOL_WIDTH:], w_dw[l], b_dw[l], conv_ln_g[l], conv_ln_b[l], w_conv_pw[l])
        y = jnp.concatenate([rmsnorm(ya, out_norm_a_g[l]), rmsnorm(yb, out_norm_b_g[l])], axis=-1)
        x = x + y @ w_out[l]
        h = rmsnorm(x, norm_xattn_g[l])
        m = rmsnorm(mem, norm_mem_g[l])
        x = x + cross_attention(h, m, w_q_mem[l], w_kv_mem[l], w_o_mem[l])
        h = rmsnorm(x, norm_ffn_g[l])
        x = x + hierarchical_moe(h, w_router_group[l], b_router_group[l], w_router_expert[l],
                                 b_router_expert[l], w_exp_gate[l], w_exp_up[l], w_exp_down[l])
    return rmsnorm(x, final_norm_g)
```

```python
import numpy as np
from contextlib import ExitStack
import concourse.bass as bass
import concourse.mybir as mybir
from concourse.bass_utils import run_bass_kernel_spmd

F32 = mybir.dt.float32
BF16 = mybir.dt.bfloat16
ALU = mybir.AluOpType
AF = mybir.ActivationFunctionType
AX = mybir.AxisListType

NCORES = 8
D = 2048
T = 1024
HALO = 32
NT = T + HALO
KC = D // 128
EPS = 1e-6
NE = 32
DE = 512
CAP = 128
C_PS, C_BDW, C_LNG, C_LNB, C_GA, C_GB, C_WDW = 0, 8, 16, 24, 32, 40, 48
NCOL = 48 + 8 * 31
NEG_BIG = -1.0e30


class R:
    __slots__ = ("name", "w", "rd")

    def __init__(self, name):
        self.name = name
        self.w = None
        self.rd = {}


class TK:
    def __init__(self, nc, es, ndma=20):
        self.nc = nc
        self.eng = {"pe": nc.tensor, "act": nc.scalar, "dve": nc.vector, "pool": nc.gpsimd, "sp": nc.sync}
        self.sem = {k: es.enter_context(nc.semaphore("S_" + k)) for k in ("pe", "act", "dve", "pool")}
        self.cnt = {k: 0 for k in self.sem}
        self.dsem = [es.enter_context(nc.semaphore(f"DS{i}")) for i in range(ndma)]
        self.dval = [0] * ndma
        self.dnext = 0
        self.known = {e: {} for e in self.eng}

    def _semobj(self, key):
        return self.sem[key] if isinstance(key, str) else self.dsem[key[1]]

    def wait(self, e, ev):
        key, val = ev
        if self.known[e].get(key, 0) >= val:
            return
        self.eng[e].wait_ge(self._semobj(key), val)
        self.known[e][key] = val

    def _deps(self, e, reads, writes, is_dma):
        for r in reads:
            if r.w is not None:
                self.wait(e, r.w)
        for r in writes:
            if r.w is not None:
                if is_dma or r.w[0] != e:
                    self.wait(e, r.w)
            for k, v in r.rd.items():
                if is_dma or k != e:
                    self.wait(e, (k, v))

    def op(self, e, fn, reads=(), writes=()):
        self._deps(e, reads, writes, False)
        inst = fn()
        self.cnt[e] += 1
        inst.then_inc(self.sem[e], 1)
        ev = (e, self.cnt[e])
        for r in reads:
            r.rd[e] = self.cnt[e]
        for r in writes:
            r.w = ev
            r.rd = {}
        return ev

    def dma(self, q, out, in_, reads=(), writes=(), fn=None):
        self._deps(q, reads, writes, True)
        s = self.dnext
        self.dnext = (self.dnext + 1) % len(self.dsem)
        if self.dval[s] > 0:
            self.wait(q, (("d", s), self.dval[s]))
        if fn is None:
            inst = self.eng[q].dma_start(out=out, in_=in_)
        else:
            inst = fn()
        self.dval[s] += 16
        inst.then_inc(self.dsem[s], 16)
        ev = (("d", s), self.dval[s])
        for r in reads:
            r.rd[("d", s)] = self.dval[s]
        for r in writes:
            r.w = ev
            r.rd = {}
        return ev

    def barrier(self):
        for e in self.eng:
            for k in self.sem:
                if self.cnt[k] > 0:
                    self.wait(e, (k, self.cnt[k]))
            for i, v in enumerate(self.dval):
                if v > 0:
                    self.wait(e, (("d", i), v))


def build_nc(stop_after="C"):
    nc = bass.Bass("TRN2", target_bir_lowering=False)

    need = {"A": {"x", "xh", "gvec", "cols", "invc", "ident", "w_in", "w_pool", "w_pw", "w_out"}}
    need["B"] = need["A"] | {"mem", "w_q", "w_kv", "w_o"}
    declared = []

    def din(name, shape, dt=F32):
        if stop_after in need and name not in need[stop_after]:
            return None
        declared.append(name)
        return nc.dram_tensor(name, list(shape), dt, kind="ExternalInput").ap()

    x_d = din("x", [T, D])
    xh_d = din("xh", [HALO, D])
    mem_d = din("mem", [256, D])
    gvec_d = din("gvec", [5, D])
    cols_d = din("cols", [128, NCOL])
    invc_d = din("invc", [128, 4 * 16])
    ident_d = din("ident", [128, 128])
    tri_d = din("tri", [128, 128])
    iota_d = din("iota", [128, 128])
    rb_d = din("rb", [36])
    w_in_d = din("w_in", [D, 3072])
    w_pool_d = din("w_pool", [4, 256, 256])
    w_pw_d = din("w_pw", [1024, 1024])
    w_out_d = din("w_out", [D, D])
    w_q_d = din("w_q", [D, D])
    w_kv_d = din("w_kv", [D, 2 * D])
    w_o_d = din("w_o", [D, D])
    wr_d = din("wr", [D, 36])
    wg_d = din("w_gate", [NE, D, DE])
    wu_d = din("w_up", [NE, D, DE])
    wd_d = din("w_down", [NE, DE, D])
    out_d = nc.dram_tensor("out", [T, D], F32, kind="ExternalOutput").ap()
    nc._declared_inputs = declared
    dbg_d = None

    with ExitStack() as es:
        tk = TK(nc, es)
        V, A, PE, PO = nc.vector, nc.scalar, nc.tensor, nc.gpsimd

        def sb(st, name, shape, dt):
            return st.enter_context(nc.sbuf_tensor("s_" + name, list(shape), dt))

        ident_b = sb(es, "ident_b", [128, 128], BF16)
        ident_f = sb(es, "ident_f", [128, 128], F32)
        ones_b = sb(es, "ones_b", [128, 128], BF16)
        ones_f = sb(es, "ones_f", [128, 128], F32)
        cols = sb(es, "cols", [128, NCOL], F32)
        Xflat = sb(es, "X", [128, 8 * D], F32)
        Xt = Xflat[:].rearrange("p (i d) -> p i d", d=D)
        stat = sb(es, "stat", [128, 64], F32)
        r_const = R("const")
        r_stat = R("stat")
        rX = [R(f"X{i}") for i in range(8)]

        psF = [es.enter_context(nc.psum_tensor(f"psF{i}", [128, 512], F32)) for i in range(6)]
        psB = [es.enter_context(nc.psum_tensor(f"psB{i}", [128, 1024], BF16)) for i in range(2)]
        rpsF = [R(f"psF{i}") for i in range(6)]
        rpsB = [R(f"psB{i}") for i in range(2)]
        pcount = {"F": 0, "B": 0}

        reserved = set()

        def ps_f(hold=False):
            while True:
                i = pcount["F"] % 6
                pcount["F"] += 1
                if i not in reserved:
                    break
            if hold:
                reserved.add(i)
            return psF[i], rpsF[i]

        def ps_release(t):
            reserved.discard(psF.index(t))

        def ps_b():
            i = pcount["B"] % 2
            pcount["B"] += 1
            return psB[i], rpsB[i]

        alt = {"n": 0}

        def evac_copy(out_ap, in_ap, reads, writes, eng=None):
            if eng is None:
                eng = "act" if alt["n"] % 2 == 0 else "dve"
                alt["n"] += 1
            if eng == "act":
                tk.op("act", lambda: A.copy(out=out_ap, in_=in_ap), reads, writes)
            else:
                tk.op("dve", lambda: V.tensor_copy(out=out_ap, in_=in_ap), reads, writes)

        tk.dma("sp", ident_f[:], ident_d, writes=[r_const])
        tk.dma("pool", ident_b[:], ident_d, writes=[r_const])
        tk.dma("sp", cols[:], cols_d, writes=[r_const])
        tk.op("dve", lambda: V.memset(ones_b[:], 1.0), writes=[r_const])
        tk.op("dve", lambda: V.memset(ones_f[:], 1.0), writes=[r_const])

        def rms_tile(src_ap, npart, gb_t, hb_ap, junk_t, reads, writes, scol, extra_reads=()):
            ssq = stat[0:npart, scol:scol + 1]
            rs = stat[0:npart, scol + 1:scol + 2]
            tk.op("act", lambda: A.activation(out=junk_t[0:npart, :], in_=src_ap, func=AF.Square, accum_out=ssq),
                  reads=list(reads), writes=[r_stat, junk_r])
            tk.op("act", lambda: A.activation(out=rs, in_=ssq, func=AF.Sqrt, scale=1.0 / D, bias=EPS),
                  reads=[r_stat], writes=[r_stat])
            tk.op("dve", lambda: V.reciprocal(out=rs, in_=rs), reads=[r_stat], writes=[r_stat])
            tk.op("dve", lambda: V.scalar_tensor_tensor(out=hb_ap, in0=src_ap, scalar=rs, in1=gb_t[0:npart, :],
                                                        op0=ALU.mult, op1=ALU.mult),
                  reads=list(reads) + [r_stat] + list(extra_reads), writes=list(writes))

        junk_r = R("junk")

        def transpose_to(hb_t, hb_r, npart, dstT, dst_r, col0, ident):
            for half in range(2):
                pt, rpt = ps_b()
                ptv = pt[:].rearrange("p (k n) -> p k n", n=128)

                def f(half=half, ptv=ptv):
                    inst = None
                    for j in range(8):
                        kc = half * 8 + j
                        inst = PE.transpose(ptv[:, j, 0:npart], hb_t[0:npart, kc * 128:(kc + 1) * 128],
                                            ident[0:npart, 0:npart])
                    return inst
                tk.op("pe", f, reads=[hb_r, r_const], writes=[rpt])
                evac_copy(dstT[:, half * 8:(half + 1) * 8, col0:col0 + npart], ptv[:, :, 0:npart],
                          reads=[rpt], writes=[dst_r])

        def load_w(slot_t, slot_r, src_ap):
            tk.dma("pool", slot_t, src_ap, writes=[slot_r])

        with ExitStack() as sA:
            hT = sb(sA, "hT", [128, KC, NT], BF16)
            r_hT = R("hT")
            wring = [sb(sA, f"wrA{i}", [128, KC, 512], BF16) for i in range(3)]
            r_wring = [R(f"wrA{i}") for i in range(3)]
            wcnt = {"n": 0}

            def wslot():
                i = wcnt["n"] % 3
                wcnt["n"] += 1
                return wring[i], r_wring[i]

            with ExitStack() as sA1:
                uT = Xflat[:, 0:8 * NT].rearrange("p (c t) -> p c t", t=NT)
                yG = Xflat[:, 8 * NT:12 * NT].bitcast(BF16).rearrange("p (c t) -> p c t", t=NT)
                r_uT = [R(f"uT{c}") for c in range(8)]
                r_yG = [R(f"yG{c}") for c in range(8)]
                with ExitStack() as sA0:
                    xin = [sb(sA0, f"xin{i}", [128, D], F32) for i in range(2)]
                    r_xin = [R(f"xin{i}") for i in range(2)]
                    hb = [sb(sA0, f"hb{i}", [128, D], BF16) for i in range(2)]
                    r_hb = [R(f"hb{i}") for i in range(2)]
                    junk = sb(sA0, "junk", [128, D], BF16)
                    gb = sb(sA0, "gb", [128, D], F32)
                    r_gb = R("gb")
                    sig = [sb(sA0, f"sig{i}", [128, 512], F32) for i in range(2)]
                    r_sig = [R(f"sig{i}") for i in range(2)]
                    tk.dma("sp", gb[:], gvec_d[0].partition_broadcast(128), writes=[r_gb])
                    for i in range(9):
                        s = i % 2
                        if i == 0:
                            npart, src, col0 = HALO, xh_d, 0
                        else:
                            npart, src, col0 = 128, x_d[(i - 1) * 128:i * 128, :], HALO + (i - 1) * 128
                        tk.dma("sp", xin[s][0:npart, :], src, writes=[r_xin[s]])
                        rms_tile(xin[s][0:npart, :], npart, gb, hb[s][0:npart, :], junk, [r_xin[s]], [r_hb[s]],
                                 scol=2 * (i % 4), extra_reads=[r_gb])
                        transpose_to(hb[s], r_hb[s], npart, hT, r_hT, col0, ident_b)
                    tchunks = [(0, 512), (512, 512), (1024, 32)]
                    for b in range(2):
                        wa, rwa = wslot()
                        load_w(wa[:], rwa, w_in_d[:, 1024 + 512 * b:1024 + 512 * (b + 1)].rearrange("(k p) n -> p k n", p=128))
                        wg_, rwg = wslot()
                        load_w(wg_[:], rwg, w_in_d[:, 2048 + 512 * b:2048 + 512 * (b + 1)].rearrange("(k p) n -> p k n", p=128))
                        for oc in range(4):
                            c = 4 * b + oc
                            for ti, (t0, n) in enumerate(tchunks):
                                pa, rpa = ps_f()
                                pg, rpg = ps_f()

                                def fa(pa=pa, w=wa, oc=oc, t0=t0, n=n):
                                    inst = None
                                    for k in range(KC):
                                        inst = PE.matmul(pa[:, 0:n], lhsT=w[:, k, oc * 128:(oc + 1) * 128],
                                                         rhs=hT[:, k, t0:t0 + n], start=(k == 0), stop=(k == KC - 1))
                                    return inst
                                tk.op("pe", fa, reads=[rwa, r_hT], writes=[rpa])
                                tk.op("pe", lambda pg=pg, oc=oc, t0=t0, n=n: fa(pg, wg_, oc, t0, n), reads=[rwg, r_hT], writes=[rpg])
                                s = (c * 3 + ti) % 2
                                tk.op("act", lambda pg=pg, s=s, n=n: A.activation(out=sig[s][:, 0:n], in_=pg[:, 0:n], func=AF.Sigmoid),
                                      reads=[rpg], writes=[r_sig[s]])
                                tk.op("dve", lambda pa=pa, s=s, c=c, t0=t0, n=n: V.tensor_tensor(
                                    out=yG[:, c, t0:t0 + n], in0=pa[:, 0:n], in1=sig[s][:, 0:n], op=ALU.mult),
                                    reads=[rpa, r_sig[s]], writes=[r_yG[c]])
                    for b in range(2):
                        wu_, rwu = wslot()
                        load_w(wu_[:], rwu, w_in_d[:, 512 * b:512 * (b + 1)].rearrange("(k p) n -> p k n", p=128))
                        for oc in range(4):
                            c = 4 * b + oc
                            for (t0, n) in tchunks:
                                pu, rpu = ps_f()

                                def fu(pu=pu, w=wu_, oc=oc, t0=t0, n=n):
                                    inst = None
                                    for k in range(KC):
                                        inst = PE.matmul(pu[:, 0:n], lhsT=w[:, k, oc * 128:(oc + 1) * 128],
                                                         rhs=hT[:, k, t0:t0 + n], start=(k == 0), stop=(k == KC - 1))
                                    return inst
                                tk.op("pe", fu, reads=[rwu, r_hT], writes=[rpu])
                                evac_copy(uT[:, c, t0:t0 + n], pu[:, 0:n], reads=[rpu], writes=[r_uT[c]])
                tk.barrier()
                if dbg_d is not None:
                    for c in range(8):
                        tk.dma("sp", dbg_d[c * 128:(c + 1) * 128, :], uT[:, c, HALO:NT], reads=[r_uT[c]])
                        tk.dma("pool", dbg_d[1024 + c * 128:1024 + (c + 1) * 128, :], yG[:, c, HALO:NT], reads=[r_yG[c]])
                    tk.barrier()
                yT = hT
                r_yT = [R(f"yT{c}") for c in range(KC)]
                with ExitStack() as sP:
                    sAB = [sb(sP, f"sAB{i}", [128, 2, NT], F32) for i in range(2)]
                    r_sAB = [R(f"sAB{i}") for i in range(2)]
                    dT = sb(sP, "dT", [128, 8, T], BF16)
                    r_dT = [R(f"dT{c}") for c in range(8)]
                    yf = sb(sP, "yf", [128, 8, 512], F32)
                    r_yf = [R(f"yfp{c}") for c in range(8)]
                    wpool = sb(sP, "wpool", [128, 4, 2, 256], BF16)
                    r_wpool = R("wpool")
                    rsb = sb(sP, "rsb", [128, 512], F32)
                    r_rsb = R("rsb")
                    sqb = [sb(sP, f"sqb{i}", [128, 512], BF16) for i in range(2)]
                    r_sqb = [R(f"sqb{i}") for i in range(2)]
                    invc = sb(sP, "invc", [128, 4, 16], F32)
                    tmpc = sb(sP, "tmpc", [128, 16], F32)
                    r_tmpc = R("tmpc")
                    r_invc = R("invc")
                    tk.dma("sp", invc[:].rearrange("p g t -> p (g t)"), invc_d, writes=[r_invc])
                    for g in range(4):
                        tk.dma("pool", wpool[:, g, :, :], w_pool_d[g].rearrange("(c p) n -> p c n", p=128), writes=[r_wpool])
                    for g in range(4):
                        w = 2 << g
                        ug = uT[:, 2 * g:2 * g + 2, :]
                        rug = [r_uT[2 * g], r_uT[2 * g + 1]]
                        cur, rcur = ug, rug
                        st = 1
                        for si in range(g + 1):
                            dst, rdst = sAB[si % 2], [r_sAB[si % 2]]
                            tk.op("dve", lambda dst=dst, cur=cur, st=st: V.tensor_tensor(
                                out=dst[:, :, st:NT], in0=cur[:, :, st:NT], in1=cur[:, :, 0:NT - st], op=ALU.add),
                                reads=rcur, writes=rdst)
                            cur, rcur = dst, rdst
                            st *= 2
                        tk.op("dve", lambda cur=cur, g=g, w=w, ug=ug: V.scalar_tensor_tensor(
                            out=dT[:, 2 * g:2 * g + 2, :], in0=cur[:, :, HALO:NT], scalar=1.0 / w, in1=ug[:, :, HALO:NT],
                            op0=ALU.mult, op1=ALU.subtract), reads=rcur + rug, writes=[r_dT[2 * g], r_dT[2 * g + 1]])
                        for c2 in range(2):
                            tk.op("dve", lambda cur=cur, g=g, c2=c2: V.tensor_tensor(
                                out=tmpc[:], in0=cur[:, c2, HALO:HALO + 16], in1=invc[:, g, :], op=ALU.mult),
                                reads=rcur + [r_invc], writes=[r_tmpc])
                            tk.op("dve", lambda g=g, c2=c2: V.tensor_tensor(
                                out=dT[:, 2 * g + c2, 0:16], in0=tmpc[:], in1=uT[:, 2 * g + c2, HALO:HALO + 16], op=ALU.subtract),
                                reads=[r_tmpc, r_uT[2 * g + c2]], writes=[r_dT[2 * g + c2]])
                    for tc in range(2):
                        t0 = tc * 512
                        pden, rpden = ps_f(hold=True)
                        for c in range(8):
                            g, oc2 = c // 2, c % 2
                            pz, rpz = ps_f()

                            def fz(pz=pz, g=g, oc2=oc2, t0=t0):
                                inst = None
                                for c2 in range(2):
                                    inst = PE.matmul(pz[:], lhsT=wpool[:, g, c2, oc2 * 128:(oc2 + 1) * 128],
                                                     rhs=dT[:, 2 * g + c2, t0:t0 + 512], start=(c2 == 0), stop=(c2 == 1))
                                return inst
                            tk.op("pe", fz, reads=[r_wpool, r_dT[2 * g], r_dT[2 * g + 1]], writes=[rpz])
                            tk.op("act", lambda pz=pz, c=c: A.activation(out=yf[:, c, :], in_=pz[:], func=AF.Copy,
                                                                         scale=cols[:, C_PS + c:C_PS + c + 1]),
                                  reads=[rpz, r_const], writes=[r_yf[c]])
                            s = c % 2
                            tk.op("dve", lambda c=c, s=s: V.tensor_tensor(out=sqb[s][:], in0=yf[:, c, :], in1=yf[:, c, :], op=ALU.mult),
                                  reads=[r_yf[c]], writes=[r_sqb[s]])
                            tk.op("pe", lambda pden=pden, s=s, c=c: PE.matmul(pden[:], lhsT=ones_b[:], rhs=sqb[s][:],
                                                                              start=(c == 0), stop=(c == 7)),
                                  reads=[r_sqb[s], r_const], writes=[rpden])
                        tk.op("act", lambda pden=pden: A.activation(out=rsb[:], in_=pden[:], func=AF.Sqrt, scale=1.0 / 1024, bias=EPS),
                              reads=[rpden], writes=[r_rsb])
                        tk.op("dve", lambda: V.reciprocal(out=rsb[:], in_=rsb[:]), reads=[r_rsb], writes=[r_rsb])
                        ps_release(pden)
                        for c in range(8):
                            tk.op("dve", lambda c=c, t0=t0: V.scalar_tensor_tensor(
                                out=yT[:, c, t0:t0 + 512], in0=yf[:, c, :], scalar=cols[:, C_GA + c:C_GA + c + 1], in1=rsb[:],
                                op0=ALU.mult, op1=ALU.mult), reads=[r_yf[c], r_rsb, r_const], writes=[r_yT[c]])
                tk.barrier()
                with ExitStack() as sC:
                    diag = [sb(sC, f"diag{i}", [128, 31, 128], BF16) for i in range(2)]
                    r_diag = [R(f"diag{i}") for i in range(2)]
                    cv = Xflat[:, 0:8 * T].rearrange("p (c t) -> p c t", t=T)
                    r_cv = [[R(f"cv{c}_{tc}") for tc in range(2)] for c in range(8)]
                    sT = sb(sC, "sT", [128, 8, 512], BF16)
                    r_sT = [R(f"sT{c}") for c in range(8)]
                    yf = sb(sC, "yfc", [128, 8, 512], F32)
                    r_yf = [R(f"yfc{c}") for c in range(8)]
                    wpw = wring[0][:].rearrange("p k n -> p (k n)").rearrange("p (k n) -> p k n", n=1024)
                    r_wpw = r_wring[0]
                    rsb = sb(sC, "rsbc", [128, 512], F32)
                    r_rsb = R("rsbc")
                    mnb = sb(sC, "mnb", [128, 512], F32)
                    r_mnb = R("mnb")
                    tmp = [sb(sC, f"tmpv{i}", [128, 512], F32) for i in range(2)]
                    r_tmp = [R(f"tmpv{i}") for i in range(2)]
                    sqb = [sb(sC, f"sqbc{i}", [128, 512], BF16) for i in range(2)]
                    r_sqb = [R(f"sqbc{i}") for i in range(2)]
                    tk.dma("pool", wpw, w_pw_d.rearrange("(k p) n -> p k n", p=128), writes=[r_wpw])
                    for c in range(8):
                        dg, rdg = diag[c % 2], r_diag[c % 2]
                        wv = cols[:, C_WDW + c * 31:C_WDW + (c + 1) * 31]
                        tk.op("dve", lambda dg=dg, wv=wv: V.tensor_tensor(
                            out=dg[:], in0=ident_f[:].unsqueeze(1).to_broadcast([128, 31, 128]),
                            in1=wv.unsqueeze(2).to_broadcast([128, 31, 128]), op=ALU.mult),
                            reads=[r_const], writes=[rdg])
                        for tc in range(2):
                            t0 = tc * 512
                            pc, rpc = ps_f()

                            def fc(pc=pc, dg=dg, c=c, t0=t0):
                                inst = None
                                for k in range(31):
                                    inst = PE.matmul(pc[:], lhsT=dg[:, k, :], rhs=yG[:, c, t0 + 2 + k:t0 + 2 + k + 512],
                                                     start=(k == 0), stop=(k == 30))
                                return inst
                            tk.op("pe", fc, reads=[rdg, r_yG[c]], writes=[rpc])
                            tk.op("act", lambda pc=pc, c=c, t0=t0: A.activation(
                                out=cv[:, c, t0:t0 + 512], in_=pc[:], func=AF.Identity, bias=cols[:, C_BDW + c:C_BDW + c + 1]),
                                reads=[rpc, r_const], writes=[r_cv[c][tc]])
                    for tc in range(2):
                        t0 = tc * 512
                        ps1, rps1 = ps_f(hold=True)
                        ps2, rps2 = ps_f(hold=True)
                        for c in range(8):
                            s = c % 2
                            tk.op("pe", lambda ps1=ps1, c=c, t0=t0: PE.matmul(ps1[:], lhsT=ones_f[:], rhs=cv[:, c, t0:t0 + 512],
                                                                              start=(c == 0), stop=(c == 7)),
                                  reads=[r_cv[c][tc], r_const], writes=[rps1])
                            tk.op("dve", lambda c=c, s=s, t0=t0: V.tensor_tensor(out=tmp[s][:], in0=cv[:, c, t0:t0 + 512],
                                                                               in1=cv[:, c, t0:t0 + 512], op=ALU.mult),
                                  reads=[r_cv[c][tc]], writes=[r_tmp[s]])
                            tk.op("pe", lambda ps2=ps2, c=c, s=s: PE.matmul(ps2[:], lhsT=ones_f[:], rhs=tmp[s][:],
                                                                            start=(c == 0), stop=(c == 7)),
                                  reads=[r_tmp[s], r_const], writes=[rps2])
                        tk.op("dve", lambda ps1=ps1: V.tensor_scalar(out=mnb[:], in0=ps1[:], scalar1=1.0 / 1024, scalar2=None, op0=ALU.mult),
                              reads=[rps1], writes=[r_mnb])
                        tk.op("dve", lambda: V.tensor_tensor(out=tmp[0][:], in0=mnb[:], in1=mnb[:], op=ALU.mult),
                              reads=[r_mnb], writes=[r_tmp[0]])
                        tk.op("dve", lambda ps2=ps2: V.scalar_tensor_tensor(out=rsb[:], in0=ps2[:], scalar=1.0 / 1024, in1=tmp[0][:],
                                                                            op0=ALU.mult, op1=ALU.subtract),
                              reads=[rps2, r_tmp[0]], writes=[r_rsb])
                        tk.op("act", lambda: A.activation(out=rsb[:], in_=rsb[:], func=AF.Sqrt, bias=EPS), reads=[r_rsb], writes=[r_rsb])
                        tk.op("dve", lambda: V.reciprocal(out=rsb[:], in_=rsb[:]), reads=[r_rsb], writes=[r_rsb])
                        for c in range(8):
                            s = c % 2
                            tk.op("dve", lambda c=c, s=s, t0=t0: V.tensor_tensor(out=tmp[s][:], in0=cv[:, c, t0:t0 + 512], in1=mnb[:], op=ALU.subtract),
                                  reads=[r_cv[c][tc], r_mnb], writes=[r_tmp[s]])
                            tk.op("dve", lambda s=s: V.tensor_tensor(out=tmp[s][:], in0=tmp[s][:], in1=rsb[:], op=ALU.mult),
                                  reads=[r_tmp[s], r_rsb], writes=[r_tmp[s]])
                            tk.op("act", lambda c=c, s=s: A.activation(out=sT[:, c, :], in_=tmp[s][:], func=AF.Silu,
                                                                       scale=cols[:, C_LNG + c:C_LNG + c + 1],
                                                                       bias=cols[:, C_LNB + c:C_LNB + c + 1]),
                                  reads=[r_tmp[s], r_const], writes=[r_sT[c]])
                        ps_release(ps1)
                        ps_release(ps2)
                        pden, rpden = ps_f(hold=True)
                        for oc in range(8):
                            pz, rpz = ps_f()

                            def fp(pz=pz, oc=oc):
                                inst = None
                                for k in range(8):
                                    inst = PE.matmul(pz[:], lhsT=wpw[:, k, oc * 128:(oc + 1) * 128], rhs=sT[:, k, :],
                                                     start=(k == 0), stop=(k == 7))
                                return inst
                            tk.op("pe", fp, reads=[r_wpw] + r_sT, writes=[rpz])
                            tk.op("act", lambda pz=pz, oc=oc: A.copy(out=yf[:, oc, :], in_=pz[:]), reads=[rpz], writes=[r_yf[oc]])
                            s = oc % 2
                            tk.op("dve", lambda oc=oc, s=s: V.tensor_tensor(out=sqb[s][:], in0=yf[:, oc, :], in1=yf[:, oc, :], op=ALU.mult),
                                  reads=[r_yf[oc]], writes=[r_sqb[s]])
                            tk.op("pe", lambda pden=pden, s=s, oc=oc: PE.matmul(pden[:], lhsT=ones_b[:], rhs=sqb[s][:],
                                                                                start=(oc == 0), stop=(oc == 7)),
                                  reads=[r_sqb[s], r_const], writes=[rpden])
                        tk.op("act", lambda pden=pden: A.activation(out=rsb[:], in_=pden[:], func=AF.Sqrt, scale=1.0 / 1024, bias=EPS),
                              reads=[rpden], writes=[r_rsb])
                        tk.op("dve", lambda: V.reciprocal(out=rsb[:], in_=rsb[:]), reads=[r_rsb], writes=[r_rsb])
                        ps_release(pden)
                        for oc in range(8):
                            tk.op("dve", lambda oc=oc, t0=t0: V.scalar_tensor_tensor(
                                out=yT[:, 8 + oc, t0:t0 + 512], in0=yf[:, oc, :], scalar=cols[:, C_GB + oc:C_GB + oc + 1], in1=rsb[:],
                                op0=ALU.mult, op1=ALU.mult), reads=[r_yf[oc], r_rsb, r_const], writes=[r_yT[8 + oc]])
                tk.barrier()
            tk.barrier()
            if dbg_d is not None:
                for c in range(16):
                    tk.dma("pool", dbg_d[2048 + c * 128:2048 + (c + 1) * 128, :], yT[:, c, 0:T], reads=[r_yT[c]])
                tk.barrier()
            for i in range(8):
                tk.dma("sp", Xt[:, i, :], x_d[i * 128:(i + 1) * 128, :], writes=[rX[i]])
            for nb in range(4):
                wo, rwo = wslot()
                load_w(wo[:], rwo, w_out_d[:, nb * 512:(nb + 1) * 512].rearrange("(k p) n -> p k n", p=128))
                for i in range(8):
                    px, rpx = ps_f()

                    def fo(px=px, wo=wo, i=i):
                        inst = None
                        for k in range(KC):
                            inst = PE.matmul(px[:], lhsT=yT[:, k, i * 128:(i + 1) * 128], rhs=wo[:, k, :],
                                             start=(k == 0), stop=(k == KC - 1))
                        return inst
                    tk.op("pe", fo, reads=[rwo] + r_yT, writes=[rpx])
                    tk.op("dve", lambda px=px, i=i, nb=nb: V.tensor_tensor(
                        out=Xt[:, i, nb * 512:(nb + 1) * 512], in0=Xt[:, i, nb * 512:(nb + 1) * 512], in1=px[:], op=ALU.add),
                        reads=[rpx, rX[i]], writes=[rX[i]])
            tk.barrier()

        if stop_after == "A":
            for i in range(8):
                tk.dma("sp", out_d[i * 128:(i + 1) * 128, :], Xt[:, i, :], reads=[rX[i]])
            tk.barrier()
            return nc
        with ExitStack() as sB:
            wringB = [sb(sB, f"wrB{i}", [128, KC, 512], BF16) for i in range(3)]
            r_wringB = [R(f"wrB{i}") for i in range(3)]
            wcntB = {"n": 0}

            def wslotB():
                i = wcntB["n"] % 3
                wcntB["n"] += 1
                return wringB[i], r_wringB[i]

            kT = sb(sB, "kT", [128, KC, 256], BF16)
            r_kT = R("kT")
            vv = sb(sB, "vv", [128, 2, D], BF16)
            r_vv = R("vv")
            h2T = sb(sB, "h2T", [128, KC, T], BF16)
            r_h2T = R("h2T")
            with ExitStack() as sB0:
                memt = [sb(sB0, f"memt{i}", [128, D], F32) for i in range(2)]
                r_memt = [R(f"memt{i}") for i in range(2)]
                hb = [sb(sB0, f"hbB{i}", [128, D], BF16) for i in range(2)]
                r_hb = [R(f"hbB{i}") for i in range(2)]
                junk = sb(sB0, "junkB", [128, D], BF16)
                gb = sb(sB0, "gbB", [128, D], F32)
                r_gb = R("gbB")
                mT = sb(sB0, "mT", [128, KC, 256], BF16)
                r_mT = R("mT")
                tk.dma("sp", gb[:], gvec_d[2].partition_broadcast(128), writes=[r_gb])
                for i in range(2):
                    tk.dma("sp", memt[i][:], mem_d[i * 128:(i + 1) * 128, :], writes=[r_memt[i]])
                    rms_tile(memt[i][:], 128, gb, hb[i][:], junk, [r_memt[i]], [r_hb[i]], scol=2 * i, extra_reads=[r_gb])
                    transpose_to(hb[i], r_hb[i], 128, mT, r_mT, i * 128, ident_b)
                for b in range(4):
                    w, rw = wslotB()
                    load_w(w[:], rw, w_kv_d[:, b * 512:(b + 1) * 512].rearrange("(k p) n -> p k n", p=128))
                    for oc in range(4):
                        pk, rpk = ps_f()

                        def fk(pk=pk, w=w, oc=oc):
                            inst = None
                            for k in range(KC):
                                inst = PE.matmul(pk[:, 0:256], lhsT=w[:, k, oc * 128:(oc + 1) * 128], rhs=mT[:, k, :],
                                                 start=(k == 0), stop=(k == KC - 1))
                            return inst
                        tk.op("pe", fk, reads=[rw, r_mT], writes=[rpk])
                        evac_copy(kT[:, 4 * b + oc, :], pk[:, 0:256], reads=[rpk], writes=[r_kT])
                for b in range(4):
                    w, rw = wslotB()
                    load_w(w[:], rw, w_kv_d[:, D + b * 512:D + (b + 1) * 512].rearrange("(k p) n -> p k n", p=128))
                    for mt in range(2):
                        pv, rpv = ps_f()

                        def fv(pv=pv, w=w, mt=mt):
                            inst = None
                            for k in range(KC):
                                inst = PE.matmul(pv[:], lhsT=mT[:, k, mt * 128:(mt + 1) * 128], rhs=w[:, k, :],
                                                 start=(k == 0), stop=(k == KC - 1))
                            return inst
                        tk.op("pe", fv, reads=[rw, r_mT], writes=[rpv])
                        evac_copy(vv[:, mt, b * 512:(b + 1) * 512], pv[:], reads=[rpv], writes=[r_vv])
                tk.dma("sp", gb[:], gvec_d[1].partition_broadcast(128), writes=[r_gb])
                for i in range(8):
                    s = i % 2
                    rms_tile(Xt[:, i, :], 128, gb, hb[s][:], junk, [rX[i]], [r_hb[s]], scol=2 * (i % 4), extra_reads=[r_gb])
                    transpose_to(hb[s], r_hb[s], 128, h2T, r_h2T, i * 128, ident_b)
            tk.barrier()
            with ExitStack() as sB1:
                qT = [sb(sB1, f"qT{i}", [128, 4, T], BF16) for i in range(2)]
                r_qT = [R(f"qT{i}") for i in range(2)]
                oT = [sb(sB1, f"oT{i}", [128, 4, T], BF16) for i in range(2)]
                r_oT = [R(f"oT{i}") for i in range(2)]
                pT = [sb(sB1, f"pT{i}", [128, 2, 512], BF16) for i in range(2)]
                r_pT = [R(f"pT{i}") for i in range(2)]
                rden = [sb(sB1, f"rden{i}", [128, 512], F32) for i in range(2)]
                r_rden = [R(f"rden{i}") for i in range(2)]
                for h in range(4):
                    q, rq = qT[h % 2], r_qT[h % 2]
                    o, ro = oT[h % 2], r_oT[h % 2]
                    w, rw = wslotB()
                    load_w(w[:], rw, w_q_d[:, h * 512:(h + 1) * 512].rearrange("(k p) n -> p k n", p=128))
                    wo, rwo = wslotB()
                    wov = wo[:].rearrange("p k n -> p (k n)").rearrange("p (k n) -> p k n", n=D)
                    load_w(wov, rwo, w_o_d[h * 512:(h + 1) * 512, :].rearrange("(k p) n -> p k n", p=128))
                    for oc in range(4):
                        for tc in range(2):
                            pq, rpq = ps_f()

                            def fq(pq=pq, w=w, oc=oc, tc=tc):
                                inst = None
                                for k in range(KC):
                                    inst = PE.matmul(pq[:], lhsT=w[:, k, oc * 128:(oc + 1) * 128],
                                                     rhs=h2T[:, k, tc * 512:(tc + 1) * 512], start=(k == 0), stop=(k == KC - 1))
                                return inst
                            tk.op("pe", fq, reads=[rw, r_h2T], writes=[rpq])
                            evac_copy(q[:, oc, tc * 512:(tc + 1) * 512], pq[:], reads=[rpq], writes=[rq])
                    for tc in range(2):
                        s = tc
                        for mc in range(2):
                            pS, rpS = ps_f()

                            def fs(pS=pS, h=h, mc=mc, tc=tc, q=q):
                                inst = None
                                for dc in range(4):
                                    inst = PE.matmul(pS[:], lhsT=kT[:, 4 * h + dc, mc * 128:(mc + 1) * 128],
                                                     rhs=q[:, dc, tc * 512:(tc + 1) * 512], start=(dc == 0), stop=(dc == 3))
                                return inst
                            tk.op("pe", fs, reads=[r_kT, rq], writes=[rpS])
                            tk.op("act", lambda pS=pS, s=s, mc=mc: A.activation(out=pT[s][:, mc, :], in_=pS[:], func=AF.Exp,
                                                                                scale=float(512 ** -0.5)),
                                  reads=[rpS], writes=[r_pT[s]])
                        pD, rpD = ps_f()

                        def fd(pD=pD, s=s):
                            inst = None
                            for mc in range(2):
                                inst = PE.matmul(pD[:], lhsT=ones_b[:], rhs=pT[s][:, mc, :], start=(mc == 0), stop=(mc == 1))
                            return inst
                        tk.op("pe", fd, reads=[r_pT[s], r_const], writes=[rpD])
                        tk.op("dve", lambda pD=pD, s=s: V.reciprocal(out=rden[s][:], in_=pD[:]), reads=[rpD], writes=[r_rden[s]])
                        for dc in range(4):
                            pO, rpO = ps_f()

                            def fo2(pO=pO, h=h, dc=dc, s=s):
                                inst = None
                                for mc in range(2):
                                    inst = PE.matmul(pO[:], lhsT=vv[:, mc, (4 * h + dc) * 128:(4 * h + dc + 1) * 128],
                                                     rhs=pT[s][:, mc, :], start=(mc == 0), stop=(mc == 1))
                                return inst
                            tk.op("pe", fo2, reads=[r_vv, r_pT[s]], writes=[rpO])
                            tk.op("dve", lambda pO=pO, o=o, dc=dc, tc=tc, s=s: V.tensor_tensor(
                                out=o[:, dc, tc * 512:(tc + 1) * 512], in0=pO[:], in1=rden[s][:], op=ALU.mult),
                                reads=[rpO, r_rden[s]], writes=[ro])
                    for i in range(8):
                        for nb in range(4):
                            px, rpx = ps_f()

                            def fx(px=px, o=o, i=i, nb=nb, wov=wov):
                                inst = None
                                for dc in range(4):
                                    inst = PE.matmul(px[:], lhsT=o[:, dc, i * 128:(i + 1) * 128],
                                                     rhs=wov[:, dc, nb * 512:(nb + 1) * 512], start=(dc == 0), stop=(dc == 3))
                                return inst
                            tk.op("pe", fx, reads=[ro, rwo], writes=[rpx])
                            tk.op("dve", lambda px=px, i=i, nb=nb: V.tensor_tensor(
                                out=Xt[:, i, nb * 512:(nb + 1) * 512], in0=Xt[:, i, nb * 512:(nb + 1) * 512], in1=px[:], op=ALU.add),
                                reads=[rpx, rX[i]], writes=[rX[i]])
            tk.barrier()

        if stop_after == "B":
            for i in range(8):
                tk.dma("sp", out_d[i * 128:(i + 1) * 128, :], Xt[:, i, :], reads=[rX[i]])
            tk.barrier()
            return nc

        with ExitStack() as sM:
            h3b = sb(sM, "h3b", [128, 8, D], BF16)
            r_h3b = [R(f"h3b{i}") for i in range(8)]
            lg = sb(sM, "lg", [128, 8, 36], F32)
            r_rt = R("route")
            W32 = sb(sM, "W32", [128, 8, 32], F32)
            sel32 = sb(sM, "sel32", [128, 8, 32], F32)
            selb = sb(sM, "selb", [128, 8, 32], BF16)
            Whl = sb(sM, "Whl", [128, 8, 32, 2], BF16)
            pos = sb(sM, "pos", [128, 8, 32], F32)
            rbb = sb(sM, "rbb", [128, 36], F32)
            sc = sb(sM, "sc", [128, 128], F32)
            scw = sb(sM, "scw", [128, 8, 32], F32)
            tri_b = sb(sM, "tri_b", [128, 128], BF16)
            iota_s = sb(sM, "iota_s", [128, 128], F32)
            tk.dma("pool", tri_b[:], tri_d, writes=[r_const])
            tk.dma("sp", iota_s[:], iota_d, writes=[r_const])
            tk.dma("sp", rbb[:], rb_d.partition_broadcast(128), writes=[r_const])
            with ExitStack() as sC1:
                h3f = [sb(sC1, f"h3f{i}", [128, D], F32) for i in range(2)]
                r_h3f = [R(f"h3f{i}") for i in range(2)]
                h3T = sb(sC1, "h3T", [128, KC, 128], F32)
                r_h3T = R("h3T")
                junk = sb(sC1, "junkC", [128, D], BF16)
                gb = sb(sC1, "gbC", [128, D], F32)
                r_gb = R("gbC")
                wr_s = sb(sC1, "wr_s", [128, KC, 36], F32)
                r_wr = R("wr_s")
                tk.dma("sp", wr_s[:], wr_d.rearrange("(k p) n -> p k n", p=128), writes=[r_wr])
                tk.dma("sp", gb[:], gvec_d[3].partition_broadcast(128), writes=[r_gb])
                for i in range(8):
                    s = i % 2
                    rms_tile(Xt[:, i, :], 128, gb, h3f[s][:], junk, [rX[i]], [r_h3f[s]], scol=2 * (i % 4), extra_reads=[r_gb])
                    tk.op("act", lambda i=i, s=s: A.copy(out=h3b[:, i, :], in_=h3f[s][:]), reads=[r_h3f[s]], writes=[r_h3b[i]])
                    for q4 in range(4):
                        pt, rpt = ps_f()
                        ptv = pt[:].rearrange("p (k n) -> p k n", n=128)

                        def ft(ptv=ptv, s=s, q4=q4):
                            inst = None
                            for j in range(4):
                                kc = q4 * 4 + j
                                inst = PE.transpose(ptv[:, j, :], h3f[s][:, kc * 128:(kc + 1) * 128], ident_f[:])
                            return inst
                        tk.op("pe", ft, reads=[r_h3f[s], r_const], writes=[rpt])
                        evac_copy(h3T[:, q4 * 4:(q4 + 1) * 4, :], ptv, reads=[rpt], writes=[r_h3T])
                    pl, rpl = ps_f()

                    def fl(pl=pl):
                        inst = None
                        for k in range(KC):
                            inst = PE.matmul(pl[:, 0:36], lhsT=h3T[:, k, :], rhs=wr_s[:, k, :], start=(k == 0), stop=(k == KC - 1))
                        return inst
                    tk.op("pe", fl, reads=[r_h3T, r_wr], writes=[rpl])
                    tk.op("dve", lambda pl=pl, i=i: V.tensor_copy(out=lg[:, i, :], in_=pl[:, 0:36]), reads=[rpl], writes=[r_rt])
            tk.barrier()
            lgb, gmax, ngmax, gmask, ge, gsum, pgc = sc[:, 0:36], sc[:, 36:37], sc[:, 37:38], sc[:, 38:42], sc[:, 42:46], sc[:, 46:47], sc[:, 47:48]
            lsel, m1, mask1, l2, m2, mask2 = sc[:, 48:56], sc[:, 56:57], sc[:, 57:65], sc[:, 65:73], sc[:, 73:74], sc[:, 74:82]
            dlt, ed, den_, w1, w2, we8, sel8 = sc[:, 82:83], sc[:, 83:84], sc[:, 84:85], sc[:, 85:86], sc[:, 86:87], sc[:, 87:95], sc[:, 95:103]

            def rv(fn):
                tk.op("dve", fn, reads=[r_rt, r_const], writes=[r_rt])

            def ra(fn):
                tk.op("act", fn, reads=[r_rt], writes=[r_rt])

            for i in range(8):
                rv(lambda i=i: V.tensor_tensor(out=lgb, in0=lg[:, i, :], in1=rbb[:], op=ALU.add))
                rv(lambda: V.tensor_reduce(out=gmax, in_=lgb[:, 0:4], axis=AX.X, op=ALU.max))
                rv(lambda: V.tensor_scalar(out=gmask, in0=lgb[:, 0:4], scalar1=gmax, scalar2=None, op0=ALU.is_equal))
                rv(lambda: V.tensor_scalar(out=ngmax, in0=gmax, scalar1=-1.0, scalar2=None, op0=ALU.mult))
                ra(lambda: A.activation(out=ge, in_=lgb[:, 0:4], func=AF.Exp, bias=ngmax, accum_out=gsum))
                rv(lambda: V.reciprocal(out=pgc, in_=gsum))
                rv(lambda: V.tensor_scalar(out=lsel, in0=lgb[:, 4:12], scalar1=gmask[:, 0:1], scalar2=None, op0=ALU.mult))
                for g in range(1, 4):
                    rv(lambda g=g: V.scalar_tensor_tensor(out=lsel, in0=lgb[:, 4 + 8 * g:12 + 8 * g], scalar=gmask[:, g:g + 1], in1=lsel,
                                                          op0=ALU.mult, op1=ALU.add))
                rv(lambda: V.tensor_reduce(out=m1, in_=lsel, axis=AX.X, op=ALU.max))
                rv(lambda: V.tensor_scalar(out=mask1, in0=lsel, scalar1=m1, scalar2=None, op0=ALU.is_equal))
                rv(lambda: V.scalar_tensor_tensor(out=l2, in0=mask1, scalar=NEG_BIG, in1=lsel, op0=ALU.mult, op1=ALU.add))
                rv(lambda: V.tensor_reduce(out=m2, in_=l2, axis=AX.X, op=ALU.max))
                rv(lambda: V.tensor_scalar(out=mask2, in0=l2, scalar1=m2, scalar2=None, op0=ALU.is_equal))
                rv(lambda: V.tensor_tensor(out=dlt, in0=m2, in1=m1, op=ALU.subtract))
                ra(lambda: A.activation(out=ed, in_=dlt, func=AF.Exp))
                rv(lambda: V.tensor_scalar(out=den_, in0=ed, scalar1=1.0, scalar2=None, op0=ALU.add))
                rv(lambda: V.reciprocal(out=w1, in_=den_))
                rv(lambda: V.tensor_tensor(out=w1, in0=w1, in1=pgc, op=ALU.mult))
                rv(lambda: V.tensor_tensor(out=w2, in0=w1, in1=ed, op=ALU.mult))
                rv(lambda: V.tensor_scalar(out=we8, in0=mask1, scalar1=w1, scalar2=None, op0=ALU.mult))
                rv(lambda: V.scalar_tensor_tensor(out=we8, in0=mask2, scalar=w2, in1=we8, op0=ALU.mult, op1=ALU.add))
                rv(lambda: V.tensor_tensor(out=sel8, in0=mask1, in1=mask2, op=ALU.add))
                for g in range(4):
                    rv(lambda i=i, g=g: V.tensor_scalar(out=W32[:, i, 8 * g:8 * g + 8], in0=we8, scalar1=gmask[:, g:g + 1], scalar2=None, op0=ALU.mult))
                    rv(lambda i=i, g=g: V.tensor_scalar(out=sel32[:, i, 8 * g:8 * g + 8], in0=sel8, scalar1=gmask[:, g:g + 1], scalar2=None, op0=ALU.mult))
            rv(lambda: V.tensor_copy(out=selb[:], in_=sel32[:]))
            rv(lambda: V.tensor_copy(out=Whl[:, :, :, 0], in_=W32[:]))
            rv(lambda: V.tensor_copy(out=scw[:], in_=Whl[:, :, :, 0]))
            rv(lambda: V.tensor_tensor(out=scw[:], in0=W32[:], in1=scw[:], op=ALU.subtract))
            rv(lambda: V.tensor_copy(out=Whl[:, :, :, 1], in_=scw[:]))
            for i in range(8):
                pp, rpp = ps_f()

                def fpos(pp=pp, i=i):
                    inst = None
                    for j in range(i):
                        inst = PE.matmul(pp[:, 0:32], lhsT=ones_b[:], rhs=selb[:, j, :], start=(j == 0), stop=False)
                    inst = PE.matmul(pp[:, 0:32], lhsT=tri_b[:], rhs=selb[:, i, :], start=(i == 0), stop=True)
                    return inst
                tk.op("pe", fpos, reads=[r_rt, r_const], writes=[rpp])
                tk.op("dve", lambda pp=pp, i=i: V.tensor_copy(out=pos[:, i, :], in_=pp[:, 0:32]), reads=[rpp, r_rt], writes=[r_rt])

            with ExitStack() as sE:
                wringC = [sb(sE, f"wrC{i}", [128, KC, 512], BF16) for i in range(3)]
                r_wringC = [R(f"wrC{i}") for i in range(3)]
                wcntC = {"n": 0}

                def wslotC():
                    i = wcntC["n"] % 3
                    wcntC["n"] += 1
                    return wringC[i], r_wringC[i]

                Pm = [sb(sE, f"Pm{i}", [128, 8, 128], BF16) for i in range(2)]
                r_Pm = [R(f"Pm{i}") for i in range(2)]
                PTs = [sb(sE, f"PTs{i}", [128, T], BF16) for i in range(4)]
                r_PTs = [R(f"PTs{i}") for i in range(4)]
                xeT = sb(sE, "xeT", [128, KC, 128], BF16)
                r_xeT = R("xeT")
                hid = [sb(sE, f"hid{i}", [128, 4, 128], BF16) for i in range(2)]
                r_hid = [R(f"hid{i}") for i in range(2)]
                ye = [sb(sE, f"ye{i}", [128, D], BF16) for i in range(4)]
                r_ye = [R(f"ye{i}") for i in range(4)]
                sg = sb(sE, "sg", [128, 512], F32)
                r_sg = R("sg")
                ws = sb(sE, "ws", [128, 8], F32)
                ws2 = sb(sE, "ws2", [128, 2], F32)
                r_ws2 = R("ws2")
                r_ws = [R(f"ws{i}") for i in range(4)]
                for e in range(NE):
                    eb = e % 4
                    wg, rwg = wslotC()
                    load_w(wg[:], rwg, wg_d[e].rearrange("(k p) n -> p k n", p=128))
                    wu, rwu = wslotC()
                    load_w(wu[:], rwu, wu_d[e].rearrange("(k p) n -> p k n", p=128))
                    wd, rwd = wslotC()
                    wdv = wd[:].rearrange("p k n -> p (k n)").rearrange("p (k n) -> p k n", n=D)
                    load_w(wdv, rwd, wd_d[e].rearrange("(k p) n -> p k n", p=128))
                    Pe, rPe = Pm[e % 2], r_Pm[e % 2]
                    for i in range(8):
                        tk.op("dve", lambda Pe=Pe, i=i, e=e: V.tensor_scalar(
                            out=Pe[:, i, :], in0=iota_s[:], scalar1=pos[:, i, e:e + 1], scalar2=sel32[:, i, e:e + 1],
                            op0=ALU.is_equal, op1=ALU.mult), reads=[r_rt, r_const], writes=[rPe])
                    pt, rpt = ps_b()
                    ptv = pt[:].rearrange("p (k n) -> p k n", n=128)

                    def fpt(ptv=ptv, Pe=Pe):
                        inst = None
                        for i in range(8):
                            inst = PE.transpose(ptv[:, i, :], Pe[:, i, :], ident_b[:])
                        return inst
                    tk.op("pe", fpt, reads=[rPe, r_const], writes=[rpt])
                    evac_copy(PTs[eb][:], pt[:], reads=[rpt], writes=[r_PTs[eb]])
                    pw_, rpw = ps_f()

                    def fws(pw_=pw_, Pe=Pe, e=e):
                        inst = None
                        for i in range(8):
                            inst = PE.matmul(pw_[:, 0:2], lhsT=Pe[:, i, :], rhs=Whl[:, i, e, :], start=(i == 0), stop=(i == 7))
                        return inst
                    tk.op("pe", fws, reads=[rPe, r_rt], writes=[rpw])
                    tk.op("dve", lambda pw_=pw_: V.tensor_copy(out=ws2[:], in_=pw_[:, 0:2]), reads=[rpw], writes=[r_ws2])
                    tk.op("dve", lambda eb=eb: V.tensor_tensor(out=ws[:, eb:eb + 1], in0=ws2[:, 0:1], in1=ws2[:, 1:2], op=ALU.add),
                          reads=[r_ws2], writes=[r_ws[eb]])
                    for q4 in range(4):
                        pg_, rpg = ps_f()
                        pgv = pg_[:].rearrange("p (k n) -> p k n", n=128)

                        def fg(pgv=pgv, q4=q4, Pe=Pe):
                            inst = None
                            for j in range(4):
                                dc = q4 * 4 + j
                                for i in range(8):
                                    inst = PE.matmul(pgv[:, j, :], lhsT=h3b[:, i, dc * 128:(dc + 1) * 128], rhs=Pe[:, i, :],
                                                     start=(i == 0), stop=(i == 7))
                            return inst
                        tk.op("pe", fg, reads=[rPe] + r_h3b, writes=[rpg])
                        evac_copy(xeT[:, q4 * 4:(q4 + 1) * 4, :], pgv, reads=[rpg], writes=[r_xeT])
                    pG, rpG = ps_f()
                    pU, rpU = ps_f()

                    def fgu(p_, w_):
                        pv_ = p_[:].rearrange("p (k n) -> p k n", n=128)
                        inst = None
                        for m in range(4):
                            for k in range(KC):
                                inst = PE.matmul(pv_[:, m, :], lhsT=w_[:, k, m * 128:(m + 1) * 128], rhs=xeT[:, k, :],
                                                 start=(k == 0), stop=(k == KC - 1))
                        return inst
                    tk.op("pe", lambda pG=pG, wg=wg: fgu(pG, wg), reads=[rwg, r_xeT], writes=[rpG])
                    tk.op("pe", lambda pU=pU, wu=wu: fgu(pU, wu), reads=[rwu, r_xeT], writes=[rpU])
                    tk.op("act", lambda pG=pG: A.activation(out=sg[:], in_=pG[:], func=AF.Silu), reads=[rpG], writes=[r_sg])
                    hd, rhd = hid[e % 2], r_hid[e % 2]
                    tk.op("dve", lambda pU=pU, hd=hd: V.tensor_tensor(out=hd[:].rearrange("p m n -> p (m n)"), in0=pU[:], in1=sg[:], op=ALU.mult),
                          reads=[rpU, r_sg], writes=[rhd])
                    for nb in range(4):
                        py, rpy = ps_f()

                        def fy(py=py, hd=hd, wdv=wdv, nb=nb):
                            inst = None
                            for m in range(4):
                                inst = PE.matmul(py[:], lhsT=hd[:, m, :], rhs=wdv[:, m, nb * 512:(nb + 1) * 512],
                                                 start=(m == 0), stop=(m == 3))
                            return inst
                        tk.op("pe", fy, reads=[rhd, rwd], writes=[rpy])
                        tk.op("act", lambda py=py, eb=eb, nb=nb: A.activation(out=ye[eb][:, nb * 512:(nb + 1) * 512], in_=py[:], func=AF.Copy,
                                                                            scale=ws[:, eb:eb + 1]),
                              reads=[rpy, r_ws[eb]], writes=[r_ye[eb]])
                    if eb == 3:
                        for i in range(8):
                            for nb in range(4):
                                pc, rpc = ps_f()

                                def fcmb(pc=pc, i=i, nb=nb):
                                    inst = None
                                    for b in range(4):
                                        inst = PE.matmul(pc[:], lhsT=PTs[b][:, i * 128:(i + 1) * 128], rhs=ye[b][:, nb * 512:(nb + 1) * 512],
                                                         start=(b == 0), stop=(b == 3))
                                    return inst
                                tk.op("pe", fcmb, reads=r_PTs + r_ye, writes=[rpc])
                                tk.op("dve", lambda pc=pc, i=i, nb=nb: V.tensor_tensor(
                                    out=Xt[:, i, nb * 512:(nb + 1) * 512], in0=Xt[:, i, nb * 512:(nb + 1) * 512], in1=pc[:], op=ALU.add),
                                    reads=[rpc, rX[i]], writes=[rX[i]])
            tk.barrier()
            with ExitStack() as sF:
                ob = [sb(sF, f"ob{i}", [128, D], F32) for i in range(2)]
                r_ob = [R(f"ob{i}") for i in range(2)]
                junk = sb(sF, "junkF", [128, D], BF16)
                gb = sb(sF, "gbF", [128, D], F32)
                r_gb = R("gbF")
                tk.dma("sp", gb[:], gvec_d[4].partition_broadcast(128), writes=[r_gb])
                for i in range(8):
                    s = i % 2
                    rms_tile(Xt[:, i, :], 128, gb, ob[s][:], junk, [rX[i]], [r_ob[s]], scol=2 * (i % 4), extra_reads=[r_gb])
                    tk.dma("sp", out_d[i * 128:(i + 1) * 128, :], ob[s][:], reads=[r_ob[s]])
                tk.barrier()
    return nc


_CONST = {}


def _consts():
    if not _CONST:
        _CONST["ident"] = np.eye(128, dtype=np.float32)
        _CONST["tri"] = np.triu(np.ones((128, 128), np.float32), 1)
        _CONST["iota"] = np.tile(np.arange(128, dtype=np.float32)[None, :], (128, 1))
    return _CONST


def _col(v, n):
    return np.ascontiguousarray(np.asarray(v, np.float32).reshape(n, 128).T)


def make_in_maps(inp):
    f = lambda a: np.ascontiguousarray(np.asarray(a, dtype=np.float32))
    x = f(inp["x"])[0]
    cols = np.zeros((128, NCOL), np.float32)
    cols[:, C_PS:C_PS + 8] = _col(inp["pool_scale"][0], 8)
    cols[:, C_BDW:C_BDW + 8] = _col(inp["b_dw"][0], 8)
    cols[:, C_LNG:C_LNG + 8] = _col(inp["conv_ln_g"][0], 8)
    cols[:, C_LNB:C_LNB + 8] = _col(inp["conv_ln_b"][0], 8)
    cols[:, C_GA:C_GA + 8] = _col(inp["out_norm_a_g"][0], 8)
    cols[:, C_GB:C_GB + 8] = _col(inp["out_norm_b_g"][0], 8)
    wdw = f(inp["w_dw"])[0, :, 0, :]
    for c in range(8):
        cols[:, C_WDW + c * 31:C_WDW + (c + 1) * 31] = wdw[:, c * 128:(c + 1) * 128].T
    gvec = np.stack([f(inp["norm_mix_g"])[0], f(inp["norm_xattn_g"])[0], f(inp["norm_mem_g"])[0],
                     f(inp["norm_ffn_g"])[0], f(inp["final_norm_g"])], 0)
    wr = np.ascontiguousarray(np.concatenate([f(inp["w_router_group"])[0], f(inp["w_router_expert"])[0]], 1))
    rb = np.ascontiguousarray(np.concatenate([f(inp["b_router_group"])[0], f(inp["b_router_expert"])[0]], 0))
    cst = _consts()
    shared = {
        "mem": f(inp["mem"])[0], "gvec": np.ascontiguousarray(gvec), "cols": cols,
        "ident": cst["ident"], "tri": cst["tri"], "iota": cst["iota"], "rb": rb,
        "w_in": f(inp["w_in"])[0], "w_pool": f(inp["w_pool"])[0], "w_pw": f(inp["w_conv_pw"])[0],
        "w_out": f(inp["w_out"])[0], "w_q": f(inp["w_q_mem"])[0], "w_kv": f(inp["w_kv_mem"])[0],
        "w_o": f(inp["w_o_mem"])[0], "wr": wr,
        "w_gate": f(inp["w_exp_gate"])[0], "w_up": f(inp["w_exp_up"])[0], "w_down": f(inp["w_exp_down"])[0],
    }
    maps = []
    for c in range(NCORES):
        m = dict(shared)
        m["x"] = np.ascontiguousarray(x[c * T:(c + 1) * T])
        if c == 0:
            m["xh"] = np.zeros((HALO, D), np.float32)
        else:
            m["xh"] = np.ascontiguousarray(x[c * T - HALO:c * T])
        invc = np.zeros((4, 16), np.float32)
        for g, w in enumerate((2, 4, 8, 16)):
            for t in range(16):
                tg = c * T + t
                invc[g, t] = 1.0 / min(tg + 1, w)
        m["invc"] = np.ascontiguousarray(np.tile(invc.reshape(1, 64), (128, 1)))
        maps.append(m)
    return maps


_NC_CACHE = {}


def kernel(**inputs):
    stop = inputs.pop("_stop_after", "C")
    if stop not in _NC_CACHE:
        _NC_CACHE[stop] = build_nc(stop)
    nc = _NC_CACHE[stop]
    maps = make_in_maps(inputs)
    maps = [{k: m[k] for k in nc._declared_inputs} for m in maps]
    res = run_bass_kernel_spmd(nc, maps, core_ids=list(range(NCORES)))
    out = np.concatenate([r["out"] for r in res.results], axis=0)
    return out.reshape(1, NCORES * T, D).astype(np.float32)
```
